# Optimizing a Trainium2 kernel written in Bass

```python
import jax, jax.numpy as jnp
from jax import lax
import numpy as np

D_MODEL = 1024
BATCH = 8
SEQ = 8192
DEPTH = 2

PLE_DIM = 256
D_FF = 2816
RMS_EPS = 1e-6
N_NORMS = 8

LRU_HEADS = 6
LRU_HEAD_DIM = 64
LRU_WIDTH = LRU_HEADS * LRU_HEAD_DIM
CONV_WIDTH = 4
LRU_C = 8.0
MLSTM_HEADS = 4
MLSTM_HEAD_DIM = 64
MLSTM_WIDTH = MLSTM_HEADS * MLSTM_HEAD_DIM
MLSTM_CHUNK = 64
RWKV_HEADS = 6
RWKV_HEAD_DIM = 64
RWKV_WIDTH = RWKV_HEADS * RWKV_HEAD_DIM
DECAY_LORA = 64
ICLR_LORA = 64
GATE_LORA = 128
GN_EPS = 64e-5

D_MIX = LRU_WIDTH + MLSTM_WIDTH + RWKV_WIDTH
RWKV_SPLITS = (RWKV_WIDTH, RWKV_WIDTH, RWKV_WIDTH, DECAY_LORA, ICLR_LORA, GATE_LORA)
N_RWKV_COLS = 3 * RWKV_WIDTH + DECAY_LORA + ICLR_LORA + GATE_LORA
MIXER_SPLITS = (LRU_WIDTH, LRU_WIDTH,
                MLSTM_WIDTH, MLSTM_WIDTH, MLSTM_WIDTH, MLSTM_WIDTH, MLSTM_HEADS, MLSTM_HEADS,
                N_RWKV_COLS)
N_IN = 2 * LRU_WIDTH + 4 * MLSTM_WIDTH + 2 * MLSTM_HEADS + N_RWKV_COLS

kernel_name = "hymba_style_lru_mlstm_rwkv7_trunk"


def _split_cols(z, sizes):
    idx = [int(s) for s in np.cumsum(sizes)[:-1]]
    return jnp.split(z, idx, axis=-1)


def _rmsnorm(x, g):
    xf = x.astype(jnp.float32)
    y = xf * lax.rsqrt(jnp.mean(xf * xf, axis=-1, keepdims=True) + RMS_EPS)
    return (y * g.astype(jnp.float32)).astype(x.dtype)


def _swiglu(h, w_in, w_out):
    gate, up = jnp.split(h @ w_in, 2, axis=-1)
    return (jax.nn.silu(gate) * up) @ w_out


def _causal_depthwise_conv(x, w, b):
    c = x.shape[-1]
    y = lax.conv_general_dilated(
        x, w[:, None, :].astype(x.dtype), window_strides=(1,),
        padding=((CONV_WIDTH - 1, 0),), dimension_numbers=("NWC", "WIO", "NWC"),
        feature_group_count=c)
    return y + b.astype(x.dtype)


def _rglru(x, w_a, b_a, w_x, b_x, lam):
    bsz, t, _ = x.shape
    f32 = jnp.float32
    xb = x.reshape(bsz, t, LRU_HEADS, LRU_HEAD_DIM)
    r = jax.nn.sigmoid((jnp.einsum("bthi,hij->bthj", xb, w_a).reshape(bsz, t, LRU_WIDTH) + b_a).astype(f32))
    i = jax.nn.sigmoid((jnp.einsum("bthi,hij->bthj", xb, w_x).reshape(bsz, t, LRU_WIDTH) + b_x).astype(f32))
    log_a = -LRU_C * r * jax.nn.softplus(-lam.astype(f32))
    a = jnp.exp(log_a)
    u = jnp.sqrt(-jnp.expm1(2.0 * log_a)) * (i * x.astype(f32))

    def combine(c1, c2):
        a1, b1 = c1
        a2, b2 = c2
        return a1 * a2, a2 * b1 + b2

    _, h = lax.associative_scan(combine, (a, u), axis=1)
    return h.astype(x.dtype)


def _mlstm_chunkwise(q, k, v, i_pre, f_pre):
    bsz, t, nh, d = q.shape
    nc = t // MLSTM_CHUNK
    f32 = jnp.float32
    def chunks(z):
        return z.astype(f32).reshape(bsz, nc, MLSTM_CHUNK, nh, d).transpose(1, 0, 3, 2, 4)
    def gchunks(z):
        return z.astype(f32).reshape(bsz, nc, MLSTM_CHUNK, nh).transpose(1, 0, 3, 2)
    qc, kc, vc = chunks(q) * (d ** -0.5), chunks(k), chunks(v)
    ic = gchunks(i_pre)
    bc = jnp.cumsum(jax.nn.log_sigmoid(gchunks(f_pre)), axis=-1)
    mask = jnp.tril(jnp.ones((MLSTM_CHUNK, MLSTM_CHUNK), bool))

    def step(carry, inp):
        c_st, n_st, m_st = carry
        qj, kj, vj, bj, ij = inp
        g = bj[..., -1]
        dmat = jnp.where(mask, bj[..., :, None] - bj[..., None, :] + ij[..., None, :], -jnp.inf)
        inter = bj + m_st[..., None]
        mj = jnp.maximum(jnp.max(dmat, axis=-1), inter)
        s = jnp.exp(dmat - mj[..., None]) * jnp.einsum("bhld,bhsd->bhls", qj, kj)
        sc = jnp.exp(inter - mj)
        num = jnp.einsum("bhls,bhsd->bhld", s, vj) + sc[..., None] * jnp.einsum("bhld,bhde->bhle", qj, c_st)
        den = jnp.sum(s, axis=-1) + sc * jnp.einsum("bhld,bhd->bhl", qj, n_st)
        h = num / jnp.maximum(jnp.abs(den), jnp.exp(-mj))[..., None]
        w_log = g[..., None] - bj + ij
        m_new = jnp.maximum(g + m_st, jnp.max(w_log, axis=-1))
        dec = jnp.exp(g + m_st - m_new)
        wk = jnp.exp(w_log - m_new[..., None])
        c_new = dec[..., None, None] * c_st + jnp.einsum("bhl,bhld,bhle->bhde", wk, kj, vj)
        n_new = dec[..., None] * n_st + jnp.einsum("bhl,bhld->bhd", wk, kj)
        return (c_new, n_new, m_new), h

    init = (jnp.zeros((bsz, nh, d, d), f32), jnp.zeros((bsz, nh, d), f32), jnp.zeros((bsz, nh), f32))
    _, h = lax.scan(step, init, (qc, kc, vc, bc, ic))
    return h.transpose(1, 0, 3, 2, 4).reshape(bsz, t, nh, d).astype(q.dtype)


def _rwkv7(zr, mu, w0, w_up, a0, a_up, g_up, k_k, k_a, r_k, ln_w, ln_b):
    bsz, t, _ = zr.shape
    f32 = jnp.float32
    prev = jnp.pad(zr, ((0, 0), (1, 0), (0, 0)))[:, :-1]
    zs = zr + (prev - zr) * mu
    r, k, v, wd, ad, gd = _split_cols(zs, RWKV_SPLITS)
    log_w = -jax.nn.softplus(-(w0 + jnp.tanh(wd) @ w_up).astype(f32)) - 0.5
    decay = jnp.exp(-jnp.exp(log_w))
    iclr = jax.nn.sigmoid((a0 + ad @ a_up).astype(f32))
    g = jax.nn.sigmoid(gd) @ g_up

    def heads(z):
        return z.astype(f32).reshape(bsz, t, RWKV_HEADS, RWKV_HEAD_DIM)

    kk = heads(k * k_k)
    kk = kk / jnp.maximum(jnp.sqrt(jnp.sum(kk * kk, axis=-1, keepdims=True)), 1e-12)
    k_mod = k.astype(f32) * (1.0 + (iclr - 1.0) * k_a.astype(f32))
    r_h, k_h, v_h, w_h, a_h = heads(r), heads(k_mod), heads(v), heads(decay), heads(iclr)
    a_vec = -kk
    b_vec = kk * a_h

    def step(s_st, inp):
        r_t, w_t, k_t, v_t, a_t, b_t = inp
        sa = jnp.einsum("bhij,bhj->bhi", s_st, a_t)
        s_st = s_st * w_t[:, :, None, :] + sa[..., None] * b_t[:, :, None, :] + v_t[..., None] * k_t[:, :, None, :]
        return s_st, jnp.einsum("bhij,bhj->bhi", s_st, r_t)

    tm = lambda z: jnp.moveaxis(z, 1, 0)
    s0 = jnp.zeros((bsz, RWKV_HEADS, RWKV_HEAD_DIM, RWKV_HEAD_DIM), f32)
    _, y = lax.scan(step, s0, (tm(r_h), tm(w_h), tm(k_h), tm(v_h), tm(a_vec), tm(b_vec)))
    y = jnp.moveaxis(y, 0, 1)
    mean = jnp.mean(y, axis=-1, keepdims=True)
    var = jnp.mean(jnp.square(y - mean), axis=-1, keepdims=True)
    yn = ((y - mean) * lax.rsqrt(var + GN_EPS)).reshape(bsz, t, RWKV_WIDTH) * ln_w.astype(f32) + ln_b.astype(f32)
    bonus = (jnp.sum(r_h * k_h * r_k.astype(f32), axis=-1, keepdims=True) * v_h).reshape(bsz, t, RWKV_WIDTH)
    return ((yn + bonus) * g.astype(f32)).astype(zr.dtype)


def _token_mixer(h, w_in, w_out, lru_conv_w, lru_conv_b, lru_w_a, lru_b_a, lru_w_x, lru_b_x, lru_lambda,
                 m_b_i, m_b_f, m_norm, rw_mu, rw_w0, rw_w_up, rw_a0, rw_a_up, rw_g_up, rw_k_k, rw_k_a,
                 rw_r_k, rw_ln_w, rw_ln_b):
    bsz, t, _ = h.shape
    z = h @ w_in
    lru_x, lru_gate, mq, mk, mv, mo, mi, mf, zr = _split_cols(z, MIXER_SPLITS)
    xa = _causal_depthwise_conv(lru_x, lru_conv_w, lru_conv_b)
    ya = _rglru(xa, lru_w_a, lru_b_a, lru_w_x, lru_b_x, lru_lambda) * jax.nn.gelu(lru_gate, approximate=True)
    mh = lambda zz: zz.reshape(bsz, t, MLSTM_HEADS, MLSTM_HEAD_DIM)
    hb = _mlstm_chunkwise(mh(mq), mh(mk), mh(mv), mi + m_b_i, mf + m_b_f)
    hb = _rmsnorm(hb, m_norm.reshape(MLSTM_HEADS, MLSTM_HEAD_DIM)).reshape(bsz, t, MLSTM_WIDTH)
    yb = (hb * jax.nn.sigmoid(mo)).astype(ya.dtype)
    yc = _rwkv7(zr, rw_mu, rw_w0, rw_w_up, rw_a0, rw_a_up, rw_g_up, rw_k_k, rw_k_a, rw_r_k,
                rw_ln_w, rw_ln_b).astype(ya.dtype)
    y = jnp.concatenate([ya, yb, yc], axis=-1)
    return y @ w_out


def setup_inputs(seed: int = 0) -> dict:
    key = jax.random.key(seed)
    ks = iter(jax.random.split(key, 48))
    f32 = jnp.float32
    def nrm(shape, scale):
        return jax.random.normal(next(ks), shape, f32) * scale
    L = DEPTH
    x = nrm((BATCH, SEQ, D_MODEL), 1.0)
    p = nrm((DEPTH, BATCH, SEQ, PLE_DIM), 1.0)
    norm_g = 1.0 + nrm((L, N_NORMS, D_MODEL), 0.05)
    ffn_w_in = nrm((L, 2, D_MODEL, 2 * D_FF), D_MODEL ** -0.5)
    ffn_w_out = nrm((L, 2, D_FF, D_MODEL), D_FF ** -0.5)
    w_in = nrm((L, D_MODEL, N_IN), D_MODEL ** -0.5)
    w_out = nrm((L, D_MIX, D_MODEL), D_MIX ** -0.5)
    lru_conv_w = nrm((L, CONV_WIDTH, LRU_WIDTH), CONV_WIDTH ** -0.5)
    lru_conv_b = nrm((L, LRU_WIDTH), 0.01)
    lru_w_a = nrm((L, LRU_HEADS, LRU_HEAD_DIM, LRU_HEAD_DIM), LRU_HEAD_DIM ** -0.5)
    lru_b_a = nrm((L, LRU_WIDTH), 0.01)
    lru_w_x = nrm((L, LRU_HEADS, LRU_HEAD_DIM, LRU_HEAD_DIM), LRU_HEAD_DIM ** -0.5)
    lru_b_x = nrm((L, LRU_WIDTH), 0.01)
    u = jax.random.uniform(next(ks), (L, LRU_WIDTH), f32, 0.9, 0.999)
    a_base = u ** (1.0 / LRU_C)
    lru_lambda = jnp.log(a_base) - jnp.log1p(-a_base)
    m_b_i = nrm((L, MLSTM_HEADS), 0.5) - 1.0
    m_b_f = jnp.linspace(3.0, 6.0, MLSTM_HEADS, dtype=f32)[None, :] + nrm((L, MLSTM_HEADS), 0.1)
    m_norm = 1.0 + nrm((L, MLSTM_WIDTH), 0.05)
    rw_mu = jax.random.uniform(next(ks), (L, N_RWKV_COLS), f32)
    rw_w0 = jax.random.uniform(next(ks), (L, RWKV_WIDTH), f32, -6.5, -1.5)
    rw_w_up = nrm((L, DECAY_LORA, RWKV_WIDTH), 0.5 * DECAY_LORA ** -0.5)
    rw_a0 = nrm((L, RWKV_WIDTH), 0.1)
    rw_a_up = nrm((L, ICLR_LORA, RWKV_WIDTH), 0.5 * ICLR_LORA ** -0.5)
    rw_g_up = nrm((L, GATE_LORA, RWKV_WIDTH), GATE_LORA ** -0.5)
    rw_k_k = 0.85 + nrm((L, RWKV_WIDTH), 0.05)
    rw_k_a = 1.0 + nrm((L, RWKV_WIDTH), 0.05)
    rw_r_k = nrm((L, RWKV_HEADS, RWKV_HEAD_DIM), 0.1)
    rw_ln_w = 1.0 + nrm((L, RWKV_WIDTH), 0.05)
    rw_ln_b = nrm((L, RWKV_WIDTH), 0.01)
    ple_w_proj = nrm((L, PLE_DIM, D_MODEL), PLE_DIM ** -0.5)
    ple_w_gate = nrm((L, D_MODEL, D_MODEL), D_MODEL ** -0.5)
    return {"x": x, "p": p, "norm_g": norm_g, "ffn_w_in": ffn_w_in, "ffn_w_out": ffn_w_out,
            "w_in": w_in, "w_out": w_out, "lru_conv_w": lru_conv_w, "lru_conv_b": lru_conv_b,
            "lru_w_a": lru_w_a, "lru_b_a": lru_b_a, "lru_w_x": lru_w_x, "lru_b_x": lru_b_x,
            "lru_lambda": lru_lambda, "m_b_i": m_b_i, "m_b_f": m_b_f, "m_norm": m_norm,
            "rw_mu": rw_mu, "rw_w0": rw_w0, "rw_w_up": rw_w_up, "rw_a0": rw_a0, "rw_a_up": rw_a_up,
            "rw_g_up": rw_g_up, "rw_k_k": rw_k_k, "rw_k_a": rw_k_a, "rw_r_k": rw_r_k,
            "rw_ln_w": rw_ln_w, "rw_ln_b": rw_ln_b, "ple_w_proj": ple_w_proj, "ple_w_gate": ple_w_gate}


def reference(x, p, norm_g, ffn_w_in, ffn_w_out, w_in, w_out, lru_conv_w, lru_conv_b, lru_w_a, lru_b_a,
              lru_w_x, lru_b_x, lru_lambda, m_b_i, m_b_f, m_norm, rw_mu, rw_w0, rw_w_up, rw_a0, rw_a_up,
              rw_g_up, rw_k_k, rw_k_a, rw_r_k, rw_ln_w, rw_ln_b, ple_w_proj, ple_w_gate):
    for l in range(DEPTH):
        g = norm_g[l]
        x = x + 0.5 * _rmsnorm(_swiglu(_rmsnorm(x, g[0]), ffn_w_in[l, 0], ffn_w_out[l, 0]), g[1])
        mix = _token_mixer(_rmsnorm(x, g[2]), w_in[l], w_out[l], lru_conv_w[l], lru_conv_b[l], lru_w_a[l],
                           lru_b_a[l], lru_w_x[l], lru_b_x[l], lru_lambda[l], m_b_i[l], m_b_f[l], m_norm[l],
                           rw_mu[l], rw_w0[l], rw_w_up[l], rw_a0[l], rw_a_up[l], rw_g_up[l], rw_k_k[l],
                           rw_k_a[l], rw_r_k[l], rw_ln_w[l], rw_ln_b[l])
        x = x + _rmsnorm(mix, g[3])
        x = x + 0.5 * _rmsnorm(_swiglu(_rmsnorm(x, g[4]), ffn_w_in[l, 1], ffn_w_out[l, 1]), g[5])
        gate = jax.nn.sigmoid(_rmsnorm(x, g[6]) @ ple_w_gate[l])
        x = x + _rmsnorm(gate * (p[l] @ ple_w_proj[l]).astype(gate.dtype), g[7])
    return x
```

```python
import numpy as np
from contextlib import ExitStack
import concourse.bass as bass
import concourse.mybir as mybir
from concourse.bass_utils import run_bass_kernel_spmd

F32 = mybir.dt.float32
BF16 = mybir.dt.bfloat16
AF = mybir.ActivationFunctionType
ALU = mybir.AluOpType
AX = mybir.AxisListType

D = 1024
SEQ = 8192
BATCH = 8
DEPTH = 2
DFF = 2816
PLE = 256
EPS = 1e-6
GN_EPS = 64e-5
NCC = 128
SEM_LIMIT = 30000


class Tile:
    __slots__ = ("name", "w", "r", "dsem")

    def __init__(self, name):
        self.name = name
        self.w = None
        self.r = {}
        self.dsem = None


class V:
    __slots__ = ("tile", "ap")

    def __init__(self, tile, ap):
        self.tile = tile
        self.ap = ap

    def __getitem__(self, k):
        return V(self.tile, self.ap[k])

    def rr(self, pat, **kw):
        return V(self.tile, self.ap.rearrange(pat, **kw))

    def bc(self, shape):
        return V(self.tile, self.ap.to_broadcast(list(shape)))

    def unsq(self, axis):
        return V(self.tile, self.ap.unsqueeze(axis))


class Op:
    __slots__ = ("eng", "fn", "deps", "dma", "sem", "val", "ms")

    def __init__(self, eng, fn, dma):
        self.eng = eng
        self.fn = fn
        self.dma = dma
        self.deps = []
        self.sem = None
        self.val = 0
        self.ms = False


class Em:
    def __init__(self, nc, gstack):
        self.nc = nc
        self.gstack = gstack
        self.engs = {"pe": nc.tensor, "act": nc.scalar, "dve": nc.vector, "pool": nc.gpsimd, "sp": nc.sync}
        self.ops = []
        self.tiles = []
        self.cur_sem = {e: None for e in self.engs}
        self.cur_cnt = {e: 0 for e in self.engs}
        self.waited = {}
        self.pending = {e: [] for e in self.engs}
        self.free_dsems = []
        self.dsem_live = []
        self.nsem = 0
        self.n_inst = 0

    def new_sem(self):
        self.nsem += 1
        return self.gstack.enter_context(self.nc.semaphore(f"s{self.nsem}"))

    def sb(self, st, name, shape, dt):
        self.uid = getattr(self, "uid", 0) + 1
        name = f"{name}_{self.uid}"
        t = st.enter_context(self.nc.sbuf_tensor(name, list(shape), dt))
        tl = Tile(name)
        self.tiles.append(tl)
        return V(tl, t[:])

    def track(self, name, ap):
        tl = Tile(name)
        self.tiles.append(tl)
        return V(tl, ap)

    def op(self, eng, fn, reads, writes, dma=False):
        o = Op(eng, fn, dma)
        deps = []
        pr = [v for v in reads if v.tile is not None and v.tile.name.startswith("G_ps")]
        if pr:
            reads = [v for v in reads if not (v.tile is not None and v.tile.name.startswith("G_ps"))]
            writes = list(writes) + pr
        for v in reads:
            t = v.tile
            if t is None:
                continue
            w = t.w
            if w is not None and not (eng == "pe" and w.eng == "pe" and not w.dma):
                deps.append(w)
        for v in writes:
            t = v.tile
            if t is None:
                continue
            w = t.w
            if w is not None:
                if w.dma and dma:
                    pass
                elif not (eng == "pe" and w.eng == "pe" and not w.dma and not dma):
                    deps.append(w)
            for r in t.r.values():
                if r is o:
                    continue
                deps.append(r)
        seen = set()
        for d in deps:
            if id(d) not in seen and d is not o:
                seen.add(id(d))
                o.deps.append(d)
        if dma:
            dt = None
            for v in (writes if writes and writes[0].tile is not None else reads):
                if v.tile is not None:
                    dt = v.tile
                    break
            if dt.dsem is None:
                if self.free_dsems:
                    dt.dsem = self.free_dsems.pop()
                else:
                    dt.dsem = [self.new_sem(), 0]
                self.dsem_live.append(dt.dsem)
            dt.dsem[1] += 16
            o.sem = dt.dsem[0]
            o.val = dt.dsem[1]
        for v in reads:
            t = v.tile
            if t is None:
                continue
            t.r[("d", id(o)) if dma else eng] = o
        for v in writes:
            t = v.tile
            if t is None:
                continue
            t.w = o
            t.r = {}
        self.ops.append(o)
        return o

    def flush(self):
        ops = self.ops
        last = {}
        for o in ops:
            for d in o.deps:
                if not d.dma:
                    d.ms = True
            if not o.dma:
                last[o.eng] = o
        for o in last.values():
            o.ms = True
        for o in ops:
            if o.ms and not o.dma:
                e = o.eng
                if self.cur_sem[e] is None or self.cur_cnt[e] >= SEM_LIMIT:
                    self.cur_sem[e] = self.new_sem()
                    self.cur_cnt[e] = 0
                self.cur_cnt[e] += 1
                o.sem = self.cur_sem[e]
                o.val = self.cur_cnt[e]
        for o in ops:
            E = self.engs[o.eng]
            waits = [(d.sem, d.val) for d in o.deps] + self.pending[o.eng]
            self.pending[o.eng] = []
            best = {}
            for sem, val in waits:
                k = id(sem)
                if k not in best or best[k][1] < val:
                    best[k] = (sem, val)
            for k, (sem, val) in best.items():
                wk = (o.eng, k)
                if self.waited.get(wk, 0) < val:
                    E.wait_ge(sem, val)
                    self.waited[wk] = val
                    self.n_inst += 1
            inst = o.fn(E)
            self.n_inst += 1
            if o.dma:
                inst.then_inc(o.sem, 16)
            elif o.ms:
                inst.then_inc(o.sem, 1)
        bar = [(o.sem, o.val) for o in last.values()]
        bar += [(d[0], d[1]) for d in self.dsem_live if d[1] > 0]
        for e in self.engs:
            self.pending[e] = self.pending[e] + list(bar)
        for t in self.tiles:
            t.w = None
            t.r = {}
            if t.dsem is not None:
                t.dsem = None
        self.free_dsems = list(self.dsem_live)
        self.dsem_live = []
        self.tiles = [t for t in self.tiles if t.name.startswith("G_")]
        self.ops = []

    def final_wait(self):
        E = self.engs["sp"]
        for sem, val in self.pending["sp"]:
            wk = ("sp", id(sem))
            if self.waited.get(wk, 0) < val:
                E.wait_ge(sem, val)
                self.waited[wk] = val

    def dma(self, out, in_, q="sp"):
        o_ap, i_ap = out.ap, in_.ap
        return self.op(q, lambda E: E.dma_start(out=o_ap, in_=i_ap), [in_], [out], dma=True)

    def mm(self, out, lhsT, rhs, start=True, stop=True):
        o, l, r = out.ap, lhsT.ap, rhs.ap
        return self.op("pe", lambda E: E.matmul(o, l, r, start=start, stop=stop), [lhsT, rhs], [out])

    def act(self, out, in_, func, bias=None, scale=None, eng="act"):
        o, i = out.ap, in_.ap
        kw = {}
        reads = [in_]
        if bias is not None:
            if isinstance(bias, V):
                kw["bias"] = bias.ap
                reads.append(bias)
            else:
                kw["bias"] = float(bias)
        if scale is not None:
            if isinstance(scale, V):
                kw["scale"] = scale.ap
                reads.append(scale)
            else:
                kw["scale"] = float(scale)
        return self.op(eng, lambda E: E.activation(out=o, in_=i, func=func, **kw), reads, [out])

    def tt(self, out, in0, in1, op, eng="dve"):
        o, a, b = out.ap, in0.ap, in1.ap
        return self.op(eng, lambda E: E.tensor_tensor(out=o, in0=a, in1=b, op=op), [in0, in1], [out])

    def ts(self, out, in0, s1, op0, s2=None, op1=None, eng="dve"):
        o, a = out.ap, in0.ap
        reads = [in0]
        if isinstance(s1, V):
            reads.append(s1)
            s1 = s1.ap
        if isinstance(s2, V):
            reads.append(s2)
            s2 = s2.ap
        if op1 is None:
            return self.op(eng, lambda E: E.tensor_scalar(out=o, in0=a, scalar1=s1, scalar2=None, op0=op0),
                           reads, [out])
        return self.op(eng, lambda E: E.tensor_scalar(out=o, in0=a, scalar1=s1, scalar2=s2, op0=op0, op1=op1),
                       reads, [out])

    def stt(self, out, in0, scalar, in1, op0, op1):
        o, a, b = out.ap, in0.ap, in1.ap
        reads = [in0, in1]
        if isinstance(scalar, V):
            reads.append(scalar)
            scalar = scalar.ap
        return self.op("dve", lambda E: E.scalar_tensor_tensor(out=o, in0=a, scalar=scalar, in1=b, op0=op0, op1=op1),
                       reads, [out])

    def copy(self, out, in_, eng="dve"):
        o, i = out.ap, in_.ap
        if eng == "act":
            return self.op("act", lambda E: E.copy(out=o, in_=i), [in_], [out])
        if eng == "dve":
            return self.op("dve", lambda E: E.tensor_scalar(out=o, in0=i, scalar1=1.0, scalar2=None, op0=ALU.mult),
                           [in_], [out])
        return self.op(eng, lambda E: E.tensor_copy(out=o, in_=i), [in_], [out])

    def recip(self, out, in_):
        o, i = out.ap, in_.ap
        return self.op("dve", lambda E: E.reciprocal(out=o, in_=i), [in_], [out])

    def memset(self, out, val, eng="pool"):
        o = out.ap
        return self.op(eng, lambda E: E.memset(o, val), [], [out])

    def scan(self, out, d0, d1, init, op0, op1):
        o, a, b = out.ap, d0.ap, d1.ap
        reads = [d0, d1]
        if isinstance(init, V):
            reads.append(init)
            init = init.ap
        return self.op("dve", lambda E: E.tensor_tensor_scan(out=o, data0=a, data1=b, initial=init, op0=op0, op1=op1),
                       reads, [out])

    def reduce(self, out, in_, op, axis=AX.X):
        o, i = out.ap, in_.ap
        return self.op("dve", lambda E: E.tensor_reduce(out=o, in_=i, axis=axis, op=op), [in_], [out])


class Prog:
    pass


def rms_rstd(E, P, ss_ps, rms_sb, rstd_sb, scale, bias_col):
    E.act(rms_sb, ss_ps, AF.Sqrt, bias=bias_col, scale=scale)
    E.recip(rstd_sb, rms_sb)


def load_cast_rows(E, P, st, dst, src_ap, nk, ncols, gcols=None, piece=1408, tag="w"):
    stg = [E.sb(st, f"stg_{tag}{i}", [128, piece], F32) for i in range(2)]
    n = 0
    for k in range(nk):
        for c0 in range(0, ncols, piece):
            w = min(piece, ncols - c0)
            s = stg[n % 2]
            E.dma(s[:, 0:w], V(None, src_ap[k * 128:(k + 1) * 128, c0:c0 + w]))
            eng = ("dve", "pool")[n % 2]
            if gcols is not None:
                E.ts(dst[:, k, c0:c0 + w], s[:, 0:w], gcols[:, k:k + 1], ALU.mult, eng=eng)
            else:
                E.copy(dst[:, k, c0:c0 + w], s[:, 0:w], eng=eng)
            n += 1


def stage_ffn(E, P, l, f, NT=256):
    T = P.T
    ps = P.ps
    npre, npost = (0, 1) if f == 0 else (4, 5)
    with ExitStack() as st:
        wi = E.sb(st, "wi", [128, 8, 2 * DFF], BF16)
        wo = E.sb(st, "wo", [128, 22, D], BF16)
        gpre = P.cc[:, l, npre * 8:npre * 8 + 8]
        gpost = P.cc[:, l, npost * 8:npost * 8 + 8]
        with ExitStack() as st2:
            load_cast_rows(E, P, st2, wi, P.ffn_w_in[l, f], 8, 2 * DFF, gcols=gpre, tag="a")
            load_cast_rows(E, P, st2, wo, P.ffn_w_out[l, f], 22, D, piece=1024, tag="b")
            E.flush()
        xt = [E.sb(st, f"xt{i}", [128, 8, NT], F32) for i in range(2)]
        sq = E.sb(st, "sq", [128, 8, NT], BF16)
        xn = E.sb(st, "xn", [128, 8, NT], BF16)
        hh = E.sb(st, "hh", [128, 22, NT], BF16)
        sg = [E.sb(st, f"sg{i}", [128, NT], F32) for i in range(2)]
        yy = E.sb(st, "yy", [128, 8, NT], F32)
        rms = E.sb(st, "rms", [128, NT], F32)
        rstd = E.sb(st, "rstd", [128, NT], F32)
        rms2 = E.sb(st, "rms2", [128, NT], F32)
        rstd2 = E.sb(st, "rstd2", [128, NT], F32)
        xo = [E.sb(st, "xo0", [128, 8, NT], F32)] * 2
        ntile = T // NT
        xsrc = P.xres.rearrange("(k p) t -> p k t", p=128)

        def load(i):
            E.dma(xt[i % 2], V(None, xsrc[:, :, i * NT:(i + 1) * NT]))

        load(0)
        for i in range(ntile):
            if i + 1 < ntile:
                load(i + 1)
            x = xt[i % 2]
            ss = ps[0][:, 0:NT]
            E.act(sq, x, AF.Square)
            for k in range(8):
                E.mm(ss, P.ones_bf, sq[:, k, :], start=(k == 0), stop=(k == 7))
            rms_rstd(E, P, ss, rms, rstd, 1.0 / D, P.eps_col)
            E.tt(xn, x, rstd.unsq(1).bc([128, 8, NT]), ALU.mult)
            for m in range(22):
                pg = ps[1 + 2 * (m % 2)][:, 0:NT]
                pu = ps[2 + 2 * (m % 2)][:, 0:NT]
                for k in range(8):
                    E.mm(pg, wi[:, k, m * 128:(m + 1) * 128], xn[:, k, :], start=(k == 0), stop=(k == 7))
                for k in range(8):
                    E.mm(pu, wi[:, k, DFF + m * 128:DFF + (m + 1) * 128], xn[:, k, :], start=(k == 0), stop=(k == 7))
                s = sg[m % 2]
                E.act(s, pg, AF.Sigmoid)
                E.tt(s, s, pg, ALU.mult)
                E.tt(hh[:, m, :], s, pu, ALU.mult)
            ss2 = ps[5][:, 0:NT]
            for c in range(8):
                py = ps[6 + (c % 2)][:, 0:NT]
                for k in range(22):
                    E.mm(py, wo[:, k, c * 128:(c + 1) * 128], hh[:, k, :], start=(k == 0), stop=(k == 21))
                E.copy(yy[:, c, :], py, eng="dve")
                E.act(sq[:, c, :], py, AF.Square)
            for c in range(8):
                E.mm(ss2, P.ones_bf, sq[:, c, :], start=(c == 0), stop=(c == 7))
            rms_rstd(E, P, ss2, rms2, rstd2, 4.0 / D, P.eps4_col)
            xout = xo[i % 2]
            E.tt(yy, yy, rstd2.unsq(1).bc([128, 8, NT]), ALU.mult)
            for c in range(8):
                E.stt(xout[:, c, :], yy[:, c, :], gpost[:, c:c + 1], x[:, c, :], ALU.mult, ALU.add)
            E.dma(V(None, xsrc[:, :, i * NT:(i + 1) * NT]), xout)
        E.flush()


def stage_ple(E, P, l, NT=256, last=False):
    T = P.T
    ps = P.ps
    with ExitStack() as st:
        wg = E.sb(st, "wg", [128, 8, D], BF16)
        wp = E.sb(st, "wp", [128, 2, D], BF16)
        gpre = P.cc[:, l, 48:56]
        gpost = P.cc[:, l, 56:64]
        with ExitStack() as st2:
            load_cast_rows(E, P, st2, wg, P.ple_w_gate[l], 8, D, gcols=gpre, piece=1024, tag="a")
            load_cast_rows(E, P, st2, wp, P.ple_w_proj[l], 2, D, piece=1024, tag="b")
            E.flush()
        xt = [E.sb(st, f"xt{i}", [128, 8, NT], F32) for i in range(2)]
        pt = [E.sb(st, f"pt{i}", [128, 2, NT], F32) for i in range(2)]
        pb = E.sb(st, "pb", [128, 2, NT], BF16)
        sq = E.sb(st, "sq", [128, 8, NT], BF16)
        xn = E.sb(st, "xn", [128, 8, NT], BF16)
        gt = [E.sb(st, f"gt{i}", [128, NT], F32) for i in range(2)]
        yy = E.sb(st, "yy", [128, 8, NT], F32)
        rms = E.sb(st, "rms", [128, NT], F32)
        rstd = E.sb(st, "rstd", [128, NT], F32)
        rms2 = E.sb(st, "rms2", [128, NT], F32)
        rstd2 = E.sb(st, "rstd2", [128, NT], F32)
        xo = [E.sb(st, f"xo{i}", [128, 8, NT], F32) for i in range(2)]
        ntile = T // NT
        xsrc = P.xres.rearrange("(k p) t -> p k t", p=128)
        xdst = (P.out if last else P.xres).rearrange("(k p) t -> p k t", p=128)
        psrc = P.pT[l].rearrange("(k p) t -> p k t", p=128)

        def load(i):
            E.dma(xt[i % 2], V(None, xsrc[:, :, i * NT:(i + 1) * NT]))
            E.dma(pt[i % 2], V(None, psrc[:, :, i * NT:(i + 1) * NT]))

        load(0)
        for i in range(ntile):
            if i + 1 < ntile:
                load(i + 1)
            x = xt[i % 2]
            ss = ps[0][:, 0:NT]
            E.act(sq, x, AF.Square)
            E.copy(pb, pt[i % 2], eng="pool")
            for k in range(8):
                E.mm(ss, P.ones_bf, sq[:, k, :], start=(k == 0), stop=(k == 7))
            rms_rstd(E, P, ss, rms, rstd, 1.0 / D, P.eps_col)
            E.tt(xn, x, rstd.unsq(1).bc([128, 8, NT]), ALU.mult)
            for c in range(8):
                pg = ps[1 + 2 * (c % 2)][:, 0:NT]
                pp = ps[2 + 2 * (c % 2)][:, 0:NT]
                for k in range(8):
                    E.mm(pg, wg[:, k, c * 128:(c + 1) * 128], xn[:, k, :], start=(k == 0), stop=(k == 7))
                for k in range(2):
                    E.mm(pp, wp[:, k, c * 128:(c + 1) * 128], pb[:, k, :], start=(k == 0), stop=(k == 1))
                g = gt[c % 2]
                E.act(g, pg, AF.Sigmoid)
                E.tt(yy[:, c, :], g, pp, ALU.mult)
                E.act(sq[:, c, :], yy[:, c, :], AF.Square)
            ss2 = ps[5][:, 0:NT]
            for c in range(8):
                E.mm(ss2, P.ones_bf, sq[:, c, :], start=(c == 0), stop=(c == 7))
            rms_rstd(E, P, ss2, rms2, rstd2, 1.0 / D, P.eps_col)
            xout = xo[i % 2]
            E.tt(yy, yy, rstd2.unsq(1).bc([128, 8, NT]), ALU.mult)
            for c in range(8):
                E.stt(xout[:, c, :], yy[:, c, :], gpost[:, c:c + 1], x[:, c, :], ALU.mult, ALU.add)
            E.dma(V(None, xdst[:, :, i * NT:(i + 1) * NT]), xout)
        E.flush()


LN8N = float(np.log(0.125))
EXPM05 = float(np.exp(-0.5))


def stage_mixer(E, P, l, NT=128):
    T = P.T
    ps = P.ps
    NCH = NT // 64
    rot = [0]

    def nb():
        rot[0] = (rot[0] + 1) % 8
        return ps[rot[0]]

    cc = P.cc
    kc = P.kcol
    one64 = kc[0:64, 2:3]
    with ExitStack() as st:
        wa = E.sb(st, "wa", [128, 8, 1800], BF16)
        wr1 = E.sb(st, "wr1", [128, 8, 1408], BF16)
        wr2 = E.sb(st, "wr2", [128, 8, 1408], BF16)
        wout = E.sb(st, "wout", [128, 8, D], BF16)
        lw = E.sb(st, "lw", [128, 1920], F32)
        rb = E.sb(st, "rb", [64, 776], F32)
        wsc = E.sb(st, "wsc", [128, 8], F32)
        cl = E.sb(st, "cl", [128, 3], F32)
        rmask = E.sb(st, "rmask", [64, NT], F32)
        gpre = cc[:, l, 16:24]
        gpost = cc[:, l, 24:32]
        with ExitStack() as st2:
            load_cast_rows(E, P, st2, wa, P.wa[l], 8, 1800, gcols=gpre, piece=1800, tag="a")
            E.memset(wsc, 1.0)
            E.copy(wsc[:, 3:5], cc[:, l, 88:90], eng="pool")
            load_cast_rows(E, P, st2, wout, P.w_out[l], 8, D, gcols=wsc, piece=1024, tag="b")
            mub = E.sb(st2, "mub", [128, 1408], F32)
            omu = E.sb(st2, "omu", [128, 1408], F32)
            E.dma(mub, V(None, P.mu_b[l]))
            E.ts(omu, mub, -1.0, ALU.mult, 1.0, ALU.add)
            zs = [E.sb(st2, f"zs{i}", [128, 1408], F32) for i in range(2)]
            for k in range(8):
                s = zs[k % 2]
                E.dma(s, V(None, P.wz[l][k * 128:(k + 1) * 128, :]))
                E.stt(wr1[:, k, :], s, gpre[:, k:k + 1], omu, ALU.mult, ALU.mult)
                E.stt(wr2[:, k, :], s, gpre[:, k:k + 1], mub, ALU.mult, ALU.mult)
            E.dma(lw, V(None, P.lw[l]))
            E.dma(rb, V(None, P.rb[l]))
            E.act(cl, cc[:, l, 85:88], AF.Exp, scale=-1.0)
            E.act(cl, cl, AF.Ln, bias=kc[:, 2:3])
            E.ts(cl, cl, -8.0, ALU.mult)
            E.memset(rmask, 1.0)
            E.memset(rmask.rr("p (n q) -> p n q", q=64)[:, :, 0:1], 0.0)
            E.flush()

        tri = P.cm_sb[:, 0, :]
        sup = P.cm_sb[:, 1, :]
        idn = P.cm_sb[:, 2, :]
        slo = P.cm_sb[:, 3, :]
        ones64 = P.ones_f[0:64, 0:64]

        xt = [E.sb(st, f"xt{i}", [128, 8, NT], F32) for i in range(2)]
        xn = E.sb(st, "xn", [128, 8, NT + 1], BF16)
        sq = E.sb(st, "sq", [128, 8, NT], BF16)
        rms = E.sb(st, "rms", [128, NT], F32)
        rstd = E.sb(st, "rstd", [128, NT], F32)
        ymix = E.sb(st, "ymix", [128, 8, NT], BF16)
        yy = E.sb(st, "yy", [128, 8, NT], F32)
        xl = E.sb(st, "xl", [128, 3, NT + 3], F32)
        hcar = E.sb(st, "hcar", [128, 3], F32)
        Cst = E.sb(st, "Cst", [64, 4, 65], F32)
        Hst = E.sb(st, "Hst", [64, 6, 64], F32)
        vext = E.sb(st, "vext", [64, NCH, 4, 65], F32)
        E.memset(xn, 0.0)
        E.memset(xl, 0.0)
        E.memset(hcar, 0.0)
        E.memset(Cst, 0.0)
        E.memset(Hst, 0.0)
        E.memset(vext, 1.0)
        lt = [E.sb(st, f"lt{i}", [128, NT], F32) for i in range(8)]
        B = [E.sb(st, f"B{i}", [64, 6 * NT], F32) for i in range(8)]
        v4 = lambda b: b[:, 0:4 * NT].rr("p (h t) -> p h t", h=4)
        vc = lambda b: b[:, 0:NCH * 256].rr("p (c f) -> p c f", c=NCH)
        va = lambda b: b[:, 0:NCH * 256].rr("p (a l) -> p a l", l=64)
        v6 = lambda b: b.rr("p (h t) -> p h t", h=6)
        qT, kT, ktok, sigo = v4(B[0]), v4(B[1]), vc(B[2]), vc(B[3])
        pre = E.sb(st, "pre", [64, NCH, 8], F32)
        nlf = E.sb(st, "nlf", [64, NCH, 4], F32)
        ibq = E.sb(st, "ibq", [64, NCH, 4], F32)
        Rall, Eq, D0, qp = va(B[4]), va(B[5]), va(B[6]), va(B[7])
        PT = Rall
        wk8 = E.sb(st, "wk8", [64, NCH * 4], F32)
        eg8 = E.sb(st, "eg8", [64, NCH * 4], F32)
        vw = E.sb(st, "vw", [64, NCH * 4, 65], F32)
        nd = E.sb(st, "nd", [64, 4, 65], F32)
        m1 = E.sb(st, "m1", [64, 4, 64], F32)
        m2 = E.sb(st, "m2", [64, 8], F32)
        m3 = E.sb(st, "m3", [64, 8], F32)
        ybt = E.sb(st, "ybt", [64, 256], F32)
        rT, kTr, nld, icl, lgn, eg, egi, w1 = [v6(b) for b in B]
        egm, kk, kmod, at, bt, kt, rt, rk = lgn, nld, kTr, lgn, icl, egi, eg, rT
        gCs = E.sb(st, "gCs", [64, 6, NCH], F32)
        twd = E.sb(st, "twd", [64, NT], F32)
        adT = E.sb(st, "adT", [64, NT], F32)
        sgd = E.sb(st, "sgd", [128, NT], F32)
        vtok = E.sb(st, "vtok", [64, NCH, 384], F32)
        gtok = E.sb(st, "gtok", [64, NCH, 384], F32)
        bon = E.sb(st, "bon", [64, NCH, 6], F32)
        Mk = [E.sb(st, f"Mk{i}", [64, 6, 64], F32) for i in range(2)]
        Nk = [E.sb(st, f"Nk{i}", [64, 6, 64], F32) for i in range(2)]
        AakT = E.sb(st, "AakT", [64, 6, 64], F32)
        ArbT = E.sb(st, "ArbT", [64, 6, 64], F32)
        ArkT = E.sb(st, "ArkT", [64, 6, 64], F32)
        Btok = E.sb(st, "Btok", [64, 6, 64], F32)
        Ktok = E.sb(st, "Ktok", [64, 6, 64], F32)
        Z = E.sb(st, "Z", [64, 6, 128], F32)
        GTs = E.sb(st, "GTs", [64, 6, 64], F32)
        N0g = E.sb(st, "N0g", [64, 6, 64], F32)
        WyT = E.sb(st, "WyT", [64, 6, 64], F32)
        ysb = E.sb(st, "ysb", [64, 6, 64], F32)
        y2 = E.sb(st, "y2", [64, 6, 64], F32)
        g1 = E.sb(st, "g1", [64, 8], F32)
        g2 = E.sb(st, "g2", [64, 8], F32)
        rms2 = E.sb(st, "rms2", [128, NT], F32)
        rstd2 = E.sb(st, "rstd2", [128, NT], F32)
        xo = yy

        ntile = T // NT
        xsrc = P.xres.rearrange("(k p) t -> p k t", p=128)

        def load(i):
            E.dma(xt[i % 2], V(None, xsrc[:, :, i * NT:(i + 1) * NT]))

        def proj_fm(out_ps, w, c0, ncol, shift):
            if not shift:
                for k in range(8):
                    E.mm(out_ps, w[:, k, c0:c0 + ncol], xn[:, k, 1:NT + 1], start=(k == 0), stop=(k == 7))
            else:
                for k in range(8):
                    E.mm(out_ps, wr1[:, k, c0:c0 + ncol], xn[:, k, 1:NT + 1], start=(k == 0), stop=False)
                for k in range(8):
                    E.mm(out_ps, wr2[:, k, c0:c0 + ncol], xn[:, k, 0:NT], start=False, stop=(k == 7))

        def proj_tm(out_ps, w, c0, ncol, ch, shift):
            t0 = 1 + ch * 64
            if not shift:
                for k in range(8):
                    E.mm(out_ps, xn[:, k, t0:t0 + 64], w[:, k, c0:c0 + ncol], start=(k == 0), stop=(k == 7))
            else:
                for k in range(8):
                    E.mm(out_ps, xn[:, k, t0:t0 + 64], wr1[:, k, c0:c0 + ncol], start=(k == 0), stop=False)
                for k in range(8):
                    E.mm(out_ps, xn[:, k, t0 - 1:t0 + 63], wr2[:, k, c0:c0 + ncol], start=False, stop=(k == 7))

        load(0)
        for i in range(ntile):
            if i + 1 < ntile:
                load(i + 1)
            x = xt[i % 2]
            ss = nb()[:, 0:NT]
            E.act(sq, x, AF.Square)
            for k in range(8):
                E.mm(ss, P.ones_bf, sq[:, k, :], start=(k == 0), stop=(k == 7))
            rms_rstd(E, P, ss, rms, rstd, 1.0 / D, kc[:, 0:1])
            E.copy(xn[:, :, 0:1], xn[:, :, NT:NT + 1], eng="pool")
            E.tt(xn[:, :, 1:NT + 1], x, rstd.unsq(1).bc([128, 8, NT]), ALU.mult)

            E.copy(xl[:, :, 0:3], xl[:, :, NT:NT + 3], eng="pool")
            for c in range(3):
                pa = nb()[:, 0:NT]
                proj_fm(pa, wa, c * 128, 128, False)
                E.copy(xl[:, c, 3:3 + NT], pa, eng="act")
                pg = nb()[:, 0:NT]
                proj_fm(pg, wa, 384 + c * 128, 128, False)
                gx, g2_, g4, gg, xa, r_, i_, a_ = lt
                E.copy(gx, pg, eng="act")
                E.tt(g2_, gx, gx, ALU.mult)
                E.ts(g2_, g2_, 0.044715, ALU.mult, 1.0, ALU.add)
                E.tt(g4, g2_, gx, ALU.mult)
                E.act(g4, g4, AF.Sigmoid, scale=1.5957691216057308)
                E.tt(gg, g4, gx, ALU.mult)
                E.ts(xa, xl[:, c, 0:NT], cc[:, l, 64 + c:65 + c], ALU.mult, cc[:, l, 76 + c:77 + c], ALU.add)
                for j in range(1, 4):
                    E.stt(xa, xl[:, c, j:j + NT], cc[:, l, 64 + j * 3 + c:65 + j * 3 + c], xa, ALU.mult, ALU.add)
                pr = nb()[:, 0:NT]
                E.mm(pr, lw[:, c * 256:c * 256 + 128], xa)
                E.act(r_, pr, AF.Sigmoid, bias=cc[:, l, 79 + c:80 + c])
                pi = nb()[:, 0:NT]
                E.mm(pi, lw[:, c * 256 + 128:c * 256 + 256], xa)
                E.act(i_, pi, AF.Sigmoid, bias=cc[:, l, 82 + c:83 + c])
                E.act(a_, r_, AF.Exp, scale=cl[:, c:c + 1])
                E.tt(r_, a_, a_, ALU.mult)
                E.act(r_, r_, AF.Sqrt, scale=-1.0, bias=kc[:, 2:3])
                E.tt(i_, i_, r_, ALU.mult)
                E.tt(i_, i_, xa, ALU.mult)
                E.scan(r_, a_, i_, hcar[:, c:c + 1], ALU.mult, ALU.add)
                E.copy(hcar[:, c:c + 1], r_[:, NT - 1:NT], eng="act")
                E.tt(ymix[:, c, :], r_, gg, ALU.mult)

            for h in range(4):
                pq = nb()[0:64, 0:NT]
                proj_fm(pq, wa, 768 + h * 64, 64, False)
                E.copy(qT[:, h, :], pq, eng="act")
                pk = nb()[0:64, 0:NT]
                proj_fm(pk, wa, 1024 + h * 64, 64, False)
                E.copy(kT[:, h, :], pk, eng="dve")
            pif = nb()
            for ch in range(NCH):
                proj_tm(pif[0:64, ch * 8:ch * 8 + 8], wa, 1792, 8, ch, False)
            E.tt(pre, pif[0:64, 0:NCH * 8].rr("p (c e) -> p c e", e=8), rb[:, 0:8].unsq(1).bc([64, NCH, 8]), ALU.add)
            for ch in range(NCH):
                tm1 = nb()[0:64, :]
                proj_tm(tm1, wa, 1024, 512, ch, False)
                E.copy(ktok[:, ch, :], tm1[:, 0:256], eng="act")
                E.copy(vext[:, ch, :, 0:64], tm1[:, 256:512].rr("p (h e) -> p h e", e=64), eng="dve")
                tm2 = nb()[0:64, 0:256]
                proj_tm(tm2, wa, 1536, 256, ch, False)
                E.act(sigo[:, ch, :], tm2, AF.Sigmoid)
            E.act(nlf, pre[:, :, 4:8], AF.Exp, scale=-1.0)
            E.act(nlf, nlf, AF.Ln, bias=one64)
            pnb = nb()[0:64, 0:NCH * 4]
            E.mm(pnb, tri, nlf.rr("p c h -> p (c h)"))
            E.stt(ibq, pnb.rr("p (c h) -> p c h", h=4), LN8N, pre[:, :, 0:4], ALU.add, ALU.add)
            E.tt(Rall, nlf.rr("p c h -> p (c h)").unsq(2).bc([64, NCH * 4, 64]),
                 tri.unsq(1).bc([64, NCH * 4, 64]), ALU.mult)
            for half in range(NCH * 4 // 8):
                pB = nb()[0:64, :]
                E.mm(pB, ones64, Rall[:, half * 8:half * 8 + 8, :].rr("p a l -> p (a l)"))
                E.act(Eq[:, half * 8:half * 8 + 8, :].rr("p a l -> p (a l)"), pB, AF.Exp, scale=-1.0, bias=kc[0:64, 3:4])
                for a in range(8):
                    idx = half * 8 + a
                    E.act(D0[:, idx, :], pB[:, a * 64:(a + 1) * 64], AF.Exp, scale=-1.0,
                          bias=ibq.rr("p c h -> p (c h)")[:, idx:idx + 1])
            E.tt(D0, D0, tri.unsq(1).bc([64, NCH * 4, 64]), ALU.mult)
            for half in range(NCH * 4 // 8):
                pS = nb()[0:64, :]
                for a in range(8):
                    idx = half * 8 + a
                    ch, h = idx // 4, idx % 4
                    E.mm(pS[:, a * 64:(a + 1) * 64], kT[:, h, ch * 64:(ch + 1) * 64], qT[:, h, ch * 64:(ch + 1) * 64])
                E.tt(PT[:, half * 8:half * 8 + 8, :].rr("p a l -> p (a l)"),
                     D0[:, half * 8:half * 8 + 8, :].rr("p a l -> p (a l)"), pS, ALU.mult)
            E.tt(qp.rr("p (c h) l -> p c h l", h=4), qT.rr("p h (c l) -> p c h l", l=64),
                 Eq.rr("p (c h) l -> p c h l", h=4), ALU.mult)
            E.ts(wk8, D0[:, :, 63], 8.0, ALU.mult)
            E.ts(eg8, Eq[:, :, 63], 8.0, ALU.mult)
            E.tt(vw, vext.rr("p c h e -> p (c h) e"), wk8.unsq(2).bc([64, NCH * 4, 65]), ALU.mult)
            for ch in range(NCH):
                pnd = nb()[0:64, 0:260].rr("p (h e) -> p h e", e=65)
                for h in range(4):
                    idx = ch * 4 + h
                    E.mm(pnd[:, h, :], PT[:, idx, :], vext[:, ch, h, :], start=True, stop=False)
                    E.mm(pnd[:, h, :], qp[:, idx, :], Cst[:, h, :], start=False, stop=True)
                pcu = nb()[0:64, 0:260].rr("p (h e) -> p h e", e=65)
                for h in range(4):
                    idx = ch * 4 + h
                    E.mm(pcu[:, h, :], ktok[:, ch, h * 64:(h + 1) * 64], vw[:, idx, :])
                E.copy(nd, pnd, eng="act")
                for h in range(4):
                    idx = ch * 4 + h
                    E.stt(Cst[:, h, :], Cst[:, h, :], eg8[:, idx:idx + 1], pcu[:, h, :], ALU.mult, ALU.add)
                dn = m2[:, 0:4]
                E.act(dn, nd[:, :, 64], AF.Abs)
                E.ts(dn, dn, 1.0, ALU.max)
                E.recip(dn, dn)
                E.tt(m1, nd[:, :, 0:64], nd[:, :, 0:64], ALU.mult)
                E.reduce(m2[:, 4:8], m1, ALU.add)
                E.tt(m2[:, 4:8], m2[:, 4:8], dn, ALU.mult)
                E.tt(m2[:, 4:8], m2[:, 4:8], dn, ALU.mult)
                E.act(m3[:, 0:4], m2[:, 4:8], AF.Sqrt, scale=1.0 / 64.0, bias=kc[0:64, 0:1])
                E.recip(m3[:, 0:4], m3[:, 0:4])
                E.tt(m3[:, 4:8], m3[:, 0:4], dn, ALU.mult)
                E.tt(m1, nd[:, :, 0:64], m3[:, 4:8].unsq(2).bc([64, 4, 64]), ALU.mult)
                E.tt(ybt.rr("p (h e) -> p h e", e=64), m1, sigo[:, ch, :].rr("p (h e) -> p h e", e=64), ALU.mult)
                for half in range(2):
                    ptr = nb()[:, 0:64]
                    E.mm(ptr, ybt[:, half * 128:(half + 1) * 128], idn)
                    E.copy(ymix[:, 3 + half, ch * 64:(ch + 1) * 64], ptr, eng="act")

            for h in range(6):
                p1 = nb()[0:64, 0:NT]
                proj_fm(p1, None, h * 64, 64, True)
                E.copy(rT[:, h, :], p1, eng="act")
                p2 = nb()[0:64, 0:NT]
                proj_fm(p2, None, 384 + h * 64, 64, True)
                E.copy(kTr[:, h, :], p2, eng="dve")
            p1 = nb()[0:64, 0:NT]
            proj_fm(p1, None, 1152, 64, True)
            E.act(twd, p1, AF.Tanh)
            p2 = nb()[0:64, 0:NT]
            proj_fm(p2, None, 1216, 64, True)
            E.copy(adT, p2, eng="act")
            p3 = nb()[:, 0:NT]
            proj_fm(p3, None, 1280, 128, True)
            E.act(sgd, p3, AF.Sigmoid)
            for ch in range(NCH):
                pv = nb()[0:64, 0:384]
                proj_tm(pv, None, 768, 384, ch, True)
                E.copy(vtok[:, ch, :], pv, eng="act")
                pgt = nb()[0:64, 0:384]
                E.mm(pgt, sgd[:, ch * 64:(ch + 1) * 64], lw[:, 1536:1920])
                E.copy(gtok[:, ch, :], pgt, eng="dve")
            for h in range(6):
                pw = nb()[0:64, 0:NT]
                E.mm(pw, lw[0:64, 768 + h * 64:832 + h * 64], twd)
                E.act(nld[:, h, :], pw, AF.Sigmoid, bias=cc[0:64, l, 90 + h:91 + h])
                pa_ = nb()[0:64, 0:NT]
                E.mm(pa_, lw[0:64, 1152 + h * 64:1216 + h * 64], adT)
                E.act(icl[:, h, :], pa_, AF.Sigmoid, bias=cc[0:64, l, 96 + h:97 + h])
            flat = lambda v: v.rr("p h t -> p (h t)")
            E.ts(flat(nld), flat(nld), EXPM05, ALU.mult)
            for h in range(6):
                E.scan(lgn[:, h, :], rmask, nld[:, h, :], 0.0, ALU.mult, ALU.add)
            E.act(flat(eg), flat(lgn), AF.Exp, scale=-1.0)
            E.act(flat(egi), flat(lgn), AF.Exp)
            E.copy(gCs, eg.rr("p h (c q) -> p h c q", q=64)[:, :, :, 63], eng="dve")
            E.tt(flat(egm), flat(lgn), flat(nld), ALU.subtract)
            E.act(flat(egm), flat(egm), AF.Exp, scale=-1.0)
            E.tt(kk, kTr, cc[0:64, l, 102:108].unsq(2).bc([64, 6, NT]), ALU.mult)
            E.tt(flat(w1), flat(kk), flat(kk), ALU.mult)
            for h in range(6):
                pn = nb()[0:64, 0:NT]
                E.mm(pn, ones64, w1[:, h, :])
                E.act(w1[:, h, :], pn, AF.Sqrt)
            E.ts(flat(w1), flat(w1), 1e-12, ALU.max)
            E.recip(flat(w1), flat(w1))
            E.tt(flat(kk), flat(kk), flat(w1), ALU.mult)
            E.ts(flat(w1), flat(icl), -1.0, ALU.add)
            E.tt(w1, w1, cc[0:64, l, 108:114].unsq(2).bc([64, 6, NT]), ALU.mult)
            E.stt(flat(kmod), flat(w1), 1.0, flat(kTr), ALU.add, ALU.mult)
            E.stt(flat(at), flat(kk), -1.0, flat(egm), ALU.mult, ALU.mult)
            E.tt(flat(bt), flat(kk), flat(icl), ALU.mult)
            E.tt(flat(bt), flat(bt), flat(egi), ALU.mult)
            E.tt(flat(kt), flat(kmod), flat(egi), ALU.mult)
            E.tt(flat(rt), flat(rT), flat(eg), ALU.mult)
            E.tt(flat(rk), flat(rT), flat(kmod), ALU.mult)
            E.tt(rk, rk, cc[0:64, l, 114:120].unsq(2).bc([64, 6, NT]), ALU.mult)
            pbn = nb()[0:64, 0:NCH * 6]
            for ch in range(NCH):
                for h in range(6):
                    E.mm(pbn[:, ch * 6 + h:ch * 6 + h + 1], rk[:, h, ch * 64:(ch + 1) * 64], P.ones_f[0:64, 0:1])
            E.copy(bon.rr("p c h -> p (c h)"), pbn, eng="act")

            for ch in range(NCH):
                cs = slice(ch * 64, (ch + 1) * 64)

                def six(out_sb, L, R, mask, op=ALU.mult, eng_copy=None):
                    pp = nb()[0:64, 0:384].rr("p (h e) -> p h e", e=64)
                    for h in range(6):
                        E.mm(pp[:, h, :], L(h), R(h))
                    if mask is None:
                        E.copy(out_sb, pp, eng=eng_copy or "act")
                    else:
                        E.tt(out_sb, pp, mask, op)

                bc6 = lambda m: m.unsq(1).bc([64, 6, 64])
                six(Mk[0], lambda h: at[:, h, cs], lambda h: bt[:, h, cs], bc6(slo))
                six(Nk[0], lambda h: bt[:, h, cs], lambda h: at[:, h, cs], bc6(sup))
                six(AakT, lambda h: kt[:, h, cs], lambda h: at[:, h, cs], bc6(sup))
                six(ArbT, lambda h: bt[:, h, cs], lambda h: rt[:, h, cs], bc6(tri))
                six(ArkT, lambda h: kt[:, h, cs], lambda h: rt[:, h, cs], bc6(tri))
                six(Z[:, :, 0:64], lambda h: at[:, h, cs], lambda h: idn, None)
                six(Btok, lambda h: bt[:, h, cs], lambda h: idn, None, eng_copy="dve")
                six(Ktok, lambda h: kt[:, h, cs], lambda h: idn, None)
                six(Z[:, :, 64:128], lambda h: AakT[:, h, :], lambda h: vtok[:, ch, h * 64:(h + 1) * 64], None,
                    eng_copy="dve")
                cur = 0
                for lev in range(6):
                    pz1 = nb()[0:64, 0:512].rr("p (h e) -> p h e", e=128)
                    for h in range(4):
                        E.mm(pz1[:, h, :], Nk[cur][:, h, :], Z[:, h, :])
                    pz2 = nb()[0:64, 0:256].rr("p (h e) -> p h e", e=128)
                    for h in range(2):
                        E.mm(pz2[:, h, :], Nk[cur][:, 4 + h, :], Z[:, 4 + h, :])
                    if lev < 5:
                        pm = nb()[0:64, 0:384].rr("p (h e) -> p h e", e=64)
                        for h in range(6):
                            E.mm(pm[:, h, :], Nk[cur][:, h, :], Mk[cur][:, h, :])
                        pn_ = nb()[0:64, 0:384].rr("p (h e) -> p h e", e=64)
                        for h in range(6):
                            E.mm(pn_[:, h, :], Mk[cur][:, h, :], Nk[cur][:, h, :])
                        E.copy(Mk[1 - cur], pm, eng="act")
                        E.copy(Nk[1 - cur], pn_, eng="act")
                    E.tt(Z[:, 0:4, :], Z[:, 0:4, :], pz1, ALU.add)
                    E.tt(Z[:, 4:6, :], Z[:, 4:6, :], pz2, ALU.add)
                    cur = 1 - cur
                Pm = lambda h: Z[:, h, 0:64]
                Qm = lambda h: Z[:, h, 64:128]
                gC = gCs[:, :, ch]
                gCb = gC.unsq(2).bc([64, 6, 64])
                six(GTs, Pm, lambda h: Btok[:, h, :], bc6(idn), op=ALU.add)
                pp = nb()[0:64, 0:384].rr("p (h e) -> p h e", e=64)
                for h in range(6):
                    E.mm(pp[:, h, :], Btok[:, h, :], Qm(h), start=True, stop=False)
                    E.mm(pp[:, h, :], Ktok[:, h, :], vtok[:, ch, h * 64:(h + 1) * 64], start=False, stop=True)
                E.tt(N0g, pp, gCb, ALU.mult)
                six(WyT, Pm, lambda h: ArbT[:, h, :], rt[:, :, cs], op=ALU.add)
                py = nb()[0:64, 0:384].rr("p (h e) -> p h e", e=64)
                for h in range(6):
                    E.mm(py[:, h, :], ArbT[:, h, :], Qm(h), start=True, stop=False)
                    E.mm(py[:, h, :], ArkT[:, h, :], vtok[:, ch, h * 64:(h + 1) * 64], start=False, stop=False)
                    E.mm(py[:, h, :], WyT[:, h, :], Hst[:, h, :], start=False, stop=True)
                phu = nb()[0:64, 0:384].rr("p (h e) -> p h e", e=64)
                for h in range(6):
                    E.mm(phu[:, h, :], GTs[:, h, :], Hst[:, h, :])
                E.copy(ysb, py, eng="act")
                E.tt(Hst, phu, gCb, ALU.mult)
                E.tt(Hst, Hst, N0g, ALU.add)
                E.reduce(g1[:, 0:6], ysb, ALU.add)
                E.ts(g1[:, 0:6], g1[:, 0:6], 1.0 / 64.0, ALU.mult)
                E.tt(ysb, ysb, g1[:, 0:6].unsq(2).bc([64, 6, 64]), ALU.subtract)
                E.tt(y2, ysb, ysb, ALU.mult)
                E.reduce(g2[:, 0:6], y2, ALU.add)
                E.act(g2[:, 0:6], g2[:, 0:6], AF.Sqrt, scale=1.0 / 64.0, bias=kc[0:64, 4:5])
                E.recip(g2[:, 0:6], g2[:, 0:6])
                E.tt(ysb, ysb, g2[:, 0:6].unsq(2).bc([64, 6, 64]), ALU.mult)
                yf = ysb.rr("p h e -> p (h e)")
                E.tt(yf, yf, rb[:, 8:392], ALU.mult)
                E.tt(yf, yf, rb[:, 392:776], ALU.add)
                E.tt(y2, vtok[:, ch, :].rr("p (h e) -> p h e", e=64), bon[:, ch, :].unsq(2).bc([64, 6, 64]), ALU.mult)
                E.tt(ysb, ysb, y2, ALU.add)
                E.tt(yf, yf, gtok[:, ch, :], ALU.mult)
                for q in range(3):
                    ptr = nb()[:, 0:64]
                    E.mm(ptr, yf[:, q * 128:(q + 1) * 128], idn)
                    E.copy(ymix[:, 5 + q, cs], ptr, eng="act")

            for c in range(8):
                py = nb()[:, 0:NT]
                for k in range(8):
                    E.mm(py, wout[:, k, c * 128:(c + 1) * 128], ymix[:, k, :], start=(k == 0), stop=(k == 7))
                E.copy(yy[:, c, :], py, eng="dve")
                E.act(sq[:, c, :], py, AF.Square)
            ss2 = nb()[:, 0:NT]
            for c in range(8):
                E.mm(ss2, P.ones_bf, sq[:, c, :], start=(c == 0), stop=(c == 7))
            rms_rstd(E, P, ss2, rms2, rstd2, 1.0 / D, kc[:, 0:1])
            E.tt(yy, yy, rstd2.unsq(1).bc([128, 8, NT]), ALU.mult)
            for c in range(8):
                E.stt(xo[:, c, :], yy[:, c, :], gpost[:, c:c + 1], x[:, c, :], ALU.mult, ALU.add)
            E.dma(V(None, xsrc[:, :, i * NT:(i + 1) * NT]), xo)
        E.flush()


def build_program(T, stages=None):
    nc = bass.Bass("TRN2", target_bir_lowering=False)
    P = Prog()
    P.T = T
    P.nc = nc
    L = DEPTH

    def din(name, shape):
        return nc.dram_tensor(name, list(shape), F32, kind="ExternalInput").ap()

    P.xT = din("xT", [D, T])
    P.pT = din("pT", [L, PLE, T])
    P.ffn_w_in = din("ffn_w_in", [L, 2, D, 2 * DFF])
    P.ffn_w_out = din("ffn_w_out", [L, 2, DFF, D])
    P.wa = din("wa", [L, D, 1800])
    P.wz = din("wz", [L, D, 1408])
    P.w_out = din("w_out", [L, D, D])
    P.ple_w_proj = din("ple_w_proj", [L, PLE, D])
    P.ple_w_gate = din("ple_w_gate", [L, D, D])
    P.cc_in = din("cc", [128, L * NCC])
    P.mu_b = din("mu_b", [L, 128, 1408])
    P.rb = din("rb", [L, 64, 8 + 384 + 384])
    P.lw = din("lw", [L, 128, 3 * 2 * 128 + 384 + 384 + 384])
    P.cm = din("cm", [64, 4 * 64])
    P.out = nc.dram_tensor("out", [D, T], F32, kind="ExternalOutput").ap()
    P.xres = nc.dram_tensor("xres", [D, T], F32).ap()
    P.mixT = nc.dram_tensor("mixT", [D, T], F32).ap()

    with ExitStack() as gstack:
        block = gstack.enter_context(nc.Block())

        @block.sync
        def _(sync):
            E = Em(nc, gstack)
            P.E = E
            global _LAST_E
            _LAST_E = E
            pst = [gstack.enter_context(nc.psum_tensor(f"ps{i}", [128, 512], F32)) for i in range(8)]
            P.ps = [E.track(f"G_ps{i}", t[:]) for i, t in enumerate(pst)]
            P.cc = E.sb(gstack, "G_cc", [128, L, NCC], F32)
            P.ones_bf = E.sb(gstack, "G_ones_bf", [128, 128], BF16)
            P.ones_f = E.sb(gstack, "G_ones_f", [128, 128], F32)
            P.eps_col = E.sb(gstack, "G_eps", [128, 1], F32)
            P.eps4_col = E.sb(gstack, "G_eps4", [128, 1], F32)
            P.cm_sb = E.sb(gstack, "G_cm", [64, 4, 64], F32)
            E.dma(P.cc, V(None, P.cc_in.rearrange("p (l c) -> p l c", l=L)))
            E.dma(P.cm_sb, V(None, P.cm.rearrange("p (a c) -> p a c", a=4)))
            E.memset(P.ones_bf, 1.0)
            E.memset(P.ones_f, 1.0)
            P.kcol = E.sb(gstack, "G_kcol", [128, 8], F32)
            for ci, cv in enumerate((EPS, 4.0 * EPS, 1.0, float(np.log(0.125)), GN_EPS)):
                E.memset(P.kcol[:, ci:ci + 1], cv)
            E.memset(P.eps_col, EPS)
            E.memset(P.eps4_col, 4.0 * EPS)
            with ExitStack() as st0:
                cp = E.sb(st0, "cp", [128, 8, 512], F32)
                for t0 in range(0, T, 512):
                    src = P.xT.rearrange("(k p) t -> p k t", p=128)[:, :, t0:t0 + 512]
                    dst = P.xres.rearrange("(k p) t -> p k t", p=128)[:, :, t0:t0 + 512]
                    E.dma(cp, V(None, src))
                    E.dma(V(None, dst), cp)
                E.flush()
            todo = stages
            if todo is None:
                todo = []
                for l in range(L):
                    todo += [("ffn", l, 0), ("mix", l), ("ffn", l, 1), ("ple", l)]
            for si, s in enumerate(todo):
                is_last = si == len(todo) - 1
                if s[0] == "ffn":
                    stage_ffn(E, P, s[1], s[2])
                elif s[0] == "mix":
                    stage_mixer(E, P, s[1])
                elif s[0] == "ple":
                    stage_ple(E, P, s[1], last=is_last)
            if not todo or todo[-1][0] != "ple":
                with ExitStack() as st0:
                    cp = E.sb(st0, "cp", [128, 8, 512], F32)
                    for t0 in range(0, T, 512):
                        src = P.xres.rearrange("(k p) t -> p k t", p=128)[:, :, t0:t0 + 512]
                        dst = P.out.rearrange("(k p) t -> p k t", p=128)[:, :, t0:t0 + 512]
                        E.dma(cp, V(None, src))
                        E.dma(V(None, dst), cp)
                    E.flush()
            E.final_wait()
    return nc


def host_pack(inp):
    L = DEPTH
    f = lambda a: np.ascontiguousarray(np.asarray(a, dtype=np.float32))
    shared = {}
    shared["ffn_w_in"] = f(inp["ffn_w_in"])
    shared["ffn_w_out"] = f(inp["ffn_w_out"])
    w_in = f(inp["w_in"])
    shared["wa"] = f(w_in[:, :, 0:1800])
    shared["wz"] = f(w_in[:, :, 1800:3208])
    shared["w_out"] = f(inp["w_out"])
    shared["ple_w_proj"] = f(inp["ple_w_proj"])
    shared["ple_w_gate"] = f(inp["ple_w_gate"])
    cc = np.zeros((128, L, NCC), np.float32)
    ng = f(inp["norm_g"])
    for l in range(L):
        for n in range(8):
            cc[:, l, n * 8:(n + 1) * 8] = ng[l, n].reshape(8, 128).T
        c = 64
        cw = f(inp["lru_conv_w"])[l]
        for j in range(4):
            cc[:, l, c + j * 3:c + j * 3 + 3] = cw[j].reshape(3, 128).T
        c += 12
        for nm in ("lru_conv_b", "lru_b_a", "lru_b_x", "lru_lambda"):
            cc[:, l, c:c + 3] = f(inp[nm])[l].reshape(3, 128).T
            c += 3
        cc[:, l, c:c + 2] = f(inp["m_norm"])[l].reshape(2, 128).T
        c += 2
        for nm in ("rw_w0", "rw_a0", "rw_k_k", "rw_k_a"):
            cc[0:64, l, c:c + 6] = f(inp[nm])[l].reshape(6, 64).T
            c += 6
        cc[0:64, l, c:c + 6] = f(inp["rw_r_k"])[l].reshape(6, 64).T
        c += 6
    shared["cc"] = f(cc.reshape(128, L * NCC))
    shared["mu_b"] = f(np.broadcast_to(f(inp["rw_mu"])[:, None, :], (L, 128, 1408)))
    rb = np.zeros((L, 64, 8 + 768), np.float32)
    rb[:, :, 0:4] = f(inp["m_b_i"])[:, None, :]
    rb[:, :, 4:8] = f(inp["m_b_f"])[:, None, :]
    rb[:, :, 8:392] = f(inp["rw_ln_w"])[:, None, :]
    rb[:, :, 392:776] = f(inp["rw_ln_b"])[:, None, :]
    shared["rb"] = rb
    lw = np.zeros((L, 128, 768 + 1152), np.float32)
    wa_, wx_ = f(inp["lru_w_a"]), f(inp["lru_w_x"])
    for l in range(L):
        for c in range(3):
            for hh in range(2):
                h = 2 * c + hh
                lw[l, hh * 64:(hh + 1) * 64, c * 256 + hh * 64:c * 256 + hh * 64 + 64] = wa_[l, h]
                lw[l, hh * 64:(hh + 1) * 64, c * 256 + 128 + hh * 64:c * 256 + 128 + hh * 64 + 64] = wx_[l, h]
        lw[l, 0:64, 768:1152] = f(inp["rw_w_up"])[l]
        lw[l, 0:64, 1152:1536] = f(inp["rw_a_up"])[l]
        lw[l, 0:128, 1536:1920] = f(inp["rw_g_up"])[l]
    shared["lw"] = lw
    cm = np.zeros((64, 4, 64), np.float32)
    ii = np.arange(64)
    cm[:, 0, :] = (ii[:, None] <= ii[None, :])
    cm[:, 1, :] = (ii[:, None] < ii[None, :])
    cm[:, 2, :] = np.eye(64)
    cm[:, 3, :] = (ii[:, None] > ii[None, :])
    shared["cm"] = f(cm.reshape(64, 256))
    return shared


_NC_CACHE = {}


def kernel(**inputs):
    x = np.asarray(inputs["x"], dtype=np.float32)
    p = np.asarray(inputs["p"], dtype=np.float32)
    B, T, _ = x.shape
    shared = host_pack(inputs)
    if T not in _NC_CACHE:
        _NC_CACHE[T] = build_program(T)
    nc = _NC_CACHE[T]
    in_maps = []
    for b in range(B):
        m = dict(shared)
        m["xT"] = np.ascontiguousarray(x[b].T)
        m["pT"] = np.ascontiguousarray(np.transpose(p[:, b], (0, 2, 1)))
        in_maps.append(m)
    res = run_bass_kernel_spmd(nc, in_maps, core_ids=list(range(B)))
    out = np.empty((B, T, D), np.float32)
    for b in range(B):
        out[b] = np.asarray(res.results[b]["out"]).T
    return out
```
